# Optimizing a Trainium2 kernel written in Bass

```python
import math
import jax, jax.numpy as jnp
from jax import lax
import numpy as np

D_MODEL = 2048
BATCH = 1
SEQ = 8192
DEPTH = 1

N_DIFF_HEADS = 8
DIFF_HEAD_DIM = D_MODEL // N_DIFF_HEADS // 2
DIFF_V_DIM = 2 * DIFF_HEAD_DIM
QK_WIDTH = 2 * N_DIFF_HEADS * DIFF_HEAD_DIM
ATTN_WIDTH = N_DIFF_HEADS * DIFF_V_DIM
ROPE_THETA = 500000.0
ROPE_DIM = DIFF_HEAD_DIM // 4
Q_BLOCK = 128
CONV_WIDTH = D_MODEL
CONV_K = 3
N_BRANCHES = 2
IN_WIDTH = 2 * QK_WIDTH + ATTN_WIDTH + 3 * CONV_WIDTH + N_BRANCHES * D_MODEL
N_EXPERTS = 32
TOP_K = 4
D_EXPERT = D_MODEL
SWIGLU_LIMIT = 7.0
SWIGLU_ALPHA = 1.702
MOE_BLOCK = 128
NORM_EPS = 1e-5
N_MOD = 6

kernel_name = 'hybrid_diffattn_shortconv_moe_block'


def rms_norm(x, g):
    x32 = x.astype(jnp.float32)
    y = x32 * lax.rsqrt(jnp.mean(x32 * x32, axis=-1, keepdims=True) + NORM_EPS)
    return (y * g.astype(jnp.float32)).astype(x.dtype)


def rope_tables(positions):
    inv_freq = ROPE_THETA ** (-jnp.arange(0, ROPE_DIM, 2, dtype=jnp.float32) / ROPE_DIM)
    ang = positions.astype(jnp.float32)[..., None] * inv_freq
    return jnp.cos(ang), jnp.sin(ang)


def apply_partial_rope(t, cos, sin):
    rot, rest = t[..., :ROPE_DIM], t[..., ROPE_DIM:]
    half = ROPE_DIM // 2
    r1, r2 = rot[..., :half], rot[..., half:]
    cc = cos[:, :, None, :].astype(t.dtype)
    ss = sin[:, :, None, :].astype(t.dtype)
    rot = jnp.concatenate([r1 * cc - r2 * ss, r2 * cc + r1 * ss], axis=-1)
    return jnp.concatenate([rot, rest], axis=-1)


def diff_attention(q, k, v, lam):
    B, S = q.shape[0], q.shape[1]
    n_blocks = S // Q_BLOCK
    scale = DIFF_HEAD_DIM ** -0.5
    k32 = k.astype(jnp.float32)
    v32 = v.astype(jnp.float32)
    qb = q.astype(jnp.float32).reshape(B, n_blocks, Q_BLOCK, 2 * N_DIFF_HEADS, DIFF_HEAD_DIM)
    qb = jnp.moveaxis(qb, 1, 0)
    key_idx = jnp.arange(S)
    neg = jnp.finfo(jnp.float32).min

    def one_block(args):
        q_blk, b_idx = args
        s = jnp.einsum('bqhd,bkhd->bhqk', q_blk, k32) * scale
        q_idx = b_idx * Q_BLOCK + jnp.arange(Q_BLOCK)
        causal = key_idx[None, :] <= q_idx[:, None]
        s = jnp.where(causal[None, None], s, neg)
        p = jax.nn.softmax(s, axis=-1).reshape(B, N_DIFF_HEADS, 2, Q_BLOCK, S)
        a = p[:, :, 0] - lam * p[:, :, 1]
        return jnp.einsum('bhqk,bkhe->bqhe', a, v32)

    o = lax.map(one_block, (qb, jnp.arange(n_blocks)))
    return jnp.moveaxis(o, 0, 1).reshape(B, S, N_DIFF_HEADS, DIFF_V_DIM)


def short_gated_conv(gate_b, gate_c, h_c, conv_w):
    u = gate_c * h_c
    conv = lax.conv_general_dilated(
        u, conv_w[:, None, :].astype(u.dtype), window_strides=(1,),
        padding=[(CONV_K - 1, 0)], dimension_numbers=('NWC', 'WIO', 'NWC'),
        feature_group_count=CONV_WIDTH)
    return gate_b * conv


def hybrid_mixer(h, cos, sin, w_in, conv_w, lq1, lk1, lq2, lk2, subln_g,
                 w_attn_out, w_conv_out, w_o, lambda_init):
    B, S, _ = h.shape
    widths = [QK_WIDTH, QK_WIDTH, ATTN_WIDTH, CONV_WIDTH, CONV_WIDTH, CONV_WIDTH, D_MODEL, D_MODEL]
    split_at = [int(v) for v in np.cumsum(widths)[:-1]]
    proj = h @ w_in
    q, k, v, cb, cc, ch, g_attn, g_conv = jnp.split(proj, split_at, axis=-1)

    q = apply_partial_rope(q.reshape(B, S, 2 * N_DIFF_HEADS, DIFF_HEAD_DIM), cos, sin)
    k = apply_partial_rope(k.reshape(B, S, 2 * N_DIFF_HEADS, DIFF_HEAD_DIM), cos, sin)
    v = v.reshape(B, S, N_DIFF_HEADS, DIFF_V_DIM)
    f32 = jnp.float32
    lam = (jnp.exp(jnp.sum(lq1.astype(f32) * lk1.astype(f32)))
           - jnp.exp(jnp.sum(lq2.astype(f32) * lk2.astype(f32))) + lambda_init)
    o = diff_attention(q, k, v, lam)
    o = rms_norm(o, subln_g) * (1.0 - lambda_init)
    y_attn = o.reshape(B, S, ATTN_WIDTH).astype(h.dtype) @ w_attn_out

    y_conv = short_gated_conv(cb, cc, ch, conv_w) @ w_conv_out

    merged = jax.nn.sigmoid(g_attn) * y_attn + jax.nn.sigmoid(g_conv) * y_conv
    return merged @ w_o


def clamped_swiglu(hup):
    x_glu, x_lin = hup[..., ::2], hup[..., 1::2]
    x_glu = jnp.minimum(x_glu, SWIGLU_LIMIT)
    x_lin = jnp.clip(x_lin, -SWIGLU_LIMIT, SWIGLU_LIMIT)
    return x_glu * jax.nn.sigmoid(SWIGLU_ALPHA * x_glu) * (x_lin + 1.0)


def moe(h, w_router, b_router, w_up, b_up, w_down, b_down):
    B, S, D = h.shape
    T = B * S
    xt = h.reshape(T, D)
    logits = (xt @ w_router).astype(jnp.float32) + b_router.astype(jnp.float32)
    top_vals, top_idx = lax.top_k(logits, TOP_K)
    top_w = jax.nn.softmax(top_vals, axis=-1)

    A = T * TOP_K
    flat_e = top_idx.reshape(A)
    flat_tok = jnp.repeat(jnp.arange(T, dtype=jnp.int32), TOP_K)
    flat_w = top_w.reshape(A)
    order = jnp.argsort(flat_e)
    se, stok, sw = flat_e[order], flat_tok[order], flat_w[order]

    counts = jnp.bincount(flat_e, length=N_EXPERTS)
    start = jnp.cumsum(counts) - counts
    padded = (counts + MOE_BLOCK - 1) // MOE_BLOCK * MOE_BLOCK
    pend = jnp.cumsum(padded)
    pstart = pend - padded
    dest = pstart[se] + (jnp.arange(A) - start[se])

    n_blocks = -(-(A + N_EXPERTS * (MOE_BLOCK - 1)) // MOE_BLOCK)
    P = n_blocks * MOE_BLOCK
    slot_tok = jnp.full((P,), T, dtype=jnp.int32).at[dest].set(stok)
    slot_w = jnp.zeros((P,), jnp.float32).at[dest].set(sw)
    block_e = jnp.minimum(
        jnp.searchsorted(pend, jnp.arange(n_blocks) * MOE_BLOCK, side='right'),
        N_EXPERTS - 1)

    x_pad = jnp.concatenate([xt, jnp.zeros((1, D), xt.dtype)], axis=0)
    xs = x_pad[slot_tok].reshape(n_blocks, MOE_BLOCK, D)

    def expert_block(args):
        xb, e = args
        hup = xb @ w_up[e] + b_up[e]
        return clamped_swiglu(hup) @ w_down[e] + b_down[e]

    ys = lax.map(expert_block, (xs, block_e)).reshape(P, D)
    out = jnp.zeros((T + 1, D), jnp.float32).at[slot_tok].add(
        ys.astype(jnp.float32) * slot_w[:, None])
    return out[:T].reshape(B, S, D).astype(h.dtype)


def setup_inputs(seed: int = 0) -> dict:
    key = jax.random.key(seed)
    ks = jax.random.split(key, 26)
    f32 = jnp.float32
    nrm = lambda k, shape, s: jax.random.normal(k, shape, f32) * s
    L = DEPTH
    return {
        'x': nrm(ks[0], (BATCH, SEQ, D_MODEL), 1.0),
        'c': nrm(ks[1], (BATCH, D_MODEL), 1.0),
        'positions': jnp.broadcast_to(jnp.arange(SEQ, dtype=jnp.int32), (BATCH, SEQ)),
        'w_ada': nrm(ks[2], (L, D_MODEL, N_MOD * D_MODEL), D_MODEL ** -0.5),
        'b_ada': nrm(ks[3], (L, N_MOD * D_MODEL), 0.02),
        'norm1_g': 1.0 + nrm(ks[4], (L, D_MODEL), 0.02),
        'w_in': nrm(ks[5], (L, D_MODEL, IN_WIDTH), D_MODEL ** -0.5),
        'conv_w': nrm(ks[6], (L, CONV_K, CONV_WIDTH), CONV_K ** -0.5),
        'lambda_q1': nrm(ks[7], (L, DIFF_HEAD_DIM), 0.1),
        'lambda_k1': nrm(ks[8], (L, DIFF_HEAD_DIM), 0.1),
        'lambda_q2': nrm(ks[9], (L, DIFF_HEAD_DIM), 0.1),
        'lambda_k2': nrm(ks[10], (L, DIFF_HEAD_DIM), 0.1),
        'subln_g': 1.0 + nrm(ks[11], (L, DIFF_V_DIM), 0.02),
        'w_attn_out': nrm(ks[12], (L, ATTN_WIDTH, D_MODEL), ATTN_WIDTH ** -0.5),
        'w_conv_out': nrm(ks[13], (L, CONV_WIDTH, D_MODEL), CONV_WIDTH ** -0.5),
        'w_o': nrm(ks[14], (L, D_MODEL, D_MODEL), D_MODEL ** -0.5),
        'norm2_g': 1.0 + nrm(ks[15], (L, D_MODEL), 0.02),
        'w_router': nrm(ks[16], (L, D_MODEL, N_EXPERTS), D_MODEL ** -0.5),
        'b_router': nrm(ks[17], (L, N_EXPERTS), 0.01),
        'w_up': nrm(ks[18], (L, N_EXPERTS, D_MODEL, 2 * D_EXPERT), D_MODEL ** -0.5),
        'b_up': nrm(ks[19], (L, N_EXPERTS, 2 * D_EXPERT), 0.02),
        'w_down': nrm(ks[20], (L, N_EXPERTS, D_EXPERT, D_MODEL), D_EXPERT ** -0.5),
        'b_down': nrm(ks[21], (L, N_EXPERTS, D_MODEL), 0.02),
        'final_g': 1.0 + nrm(ks[22], (D_MODEL,), 0.02),
    }


def reference(x, c, positions, w_ada, b_ada, norm1_g, w_in, conv_w, lambda_q1, lambda_k1,
              lambda_q2, lambda_k2, subln_g, w_attn_out, w_conv_out, w_o, norm2_g,
              w_router, b_router, w_up, b_up, w_down, b_down, final_g):
    cos, sin = rope_tables(positions)
    c_act = jax.nn.silu(c)
    for l in range(DEPTH):
        lambda_init = 0.8 - 0.6 * math.exp(-0.3 * l)
        mod = c_act @ w_ada[l] + b_ada[l]
        sh1, sc1, g1, sh2, sc2, g2 = [m[:, None, :] for m in jnp.split(mod, N_MOD, axis=-1)]
        h = rms_norm(x, norm1_g[l]) * (1.0 + sc1) + sh1
        x = x + g1 * hybrid_mixer(h, cos, sin, w_in[l], conv_w[l], lambda_q1[l], lambda_k1[l],
                                  lambda_q2[l], lambda_k2[l], subln_g[l], w_attn_out[l],
                                  w_conv_out[l], w_o[l], lambda_init)
        h = rms_norm(x, norm2_g[l]) * (1.0 + sc2) + sh2
        x = x + g2 * moe(h, w_router[l], b_router[l], w_up[l], b_up[l], w_down[l], b_down[l])
    return rms_norm(x, final_g)
```

```python
import math
from contextlib import ExitStack
import numpy as np
import concourse.bass as bass
import concourse.mybir as mybir
from concourse.bass_utils import run_bass_kernel_spmd

F32 = mybir.dt.float32
F32R = mybir.dt.float32r
BF16 = mybir.dt.bfloat16
I32 = mybir.dt.int32
AF = mybir.ActivationFunctionType
ALU = mybir.AluOpType

P = 128
D = 2048
KC = 16
NT = 1024
NS = 8
NE = 32
CAP = 256
EPS = 1e-5
LAMBDA_INIT = 0.8 - 0.6
SCALE = 128 ** -0.5
EPOCH = 16000
DMA_EPOCH = 1000


class Sched:
    ENGS = ("pe", "act", "dve", "pool", "sp")

    def __init__(self, nc, n_dma_sems=8, same_engine_sync=True):
        self.nc = nc
        self.same_engine_sync = same_engine_sync
        self.prog = {e: [] for e in self.ENGS}
        self.count = {e: 0 for e in self.ENGS}
        self.esems = {e: [] for e in self.ENGS}
        self.seen = {e: {} for e in self.ENGS}
        self.lastw = {}
        self.readers = {}
        self.sem_objs = {}
        self._sem_ctx = []
        self.dma_pool = {}
        self.n_dma_sems = n_dma_sems

    def _new_sem(self, name):
        ctx = self.nc.semaphore(name)
        s = ctx.__enter__()
        self._sem_ctx.append(ctx)
        self.sem_objs[name] = s
        return name

    def close(self):
        for ctx in reversed(self._sem_ctx):
            ctx.__exit__(None, None, None)

    def _engine_token(self, eng):
        k = self.count[eng]
        ep = k // EPOCH
        while len(self.esems[eng]) <= ep:
            self.esems[eng].append(self._new_sem(f"s_{eng}_{len(self.esems[eng])}"))
        self.count[eng] = k + 1
        return (self.esems[eng][ep], (k % EPOCH) + 1, eng)

    def _dma_token(self, eng):
        pool = self.dma_pool.setdefault(eng, {"sems": [], "uses": [], "next": 0})
        i = pool["next"]
        pool["next"] = (i + 1) % self.n_dma_sems
        if len(pool["sems"]) <= i:
            pool["sems"].append([self._new_sem(f"d_{eng}_{i}_0")])
            pool["uses"].append(0)
        j = pool["uses"][i]
        ep = j // DMA_EPOCH
        while len(pool["sems"][i]) <= ep:
            pool["sems"][i].append(self._new_sem(f"d_{eng}_{i}_{len(pool['sems'][i])}"))
        pool["uses"][i] = j + 1
        prev = None
        if j % DMA_EPOCH > 0:
            prev = (pool["sems"][i][ep], 16 * (j % DMA_EPOCH), "dma")
        elif j > 0:
            prev = (pool["sems"][i][ep - 1], 16 * DMA_EPOCH, "dma")
        return (pool["sems"][i][ep], 16 * ((j % DMA_EPOCH) + 1), "dma"), prev

    def _need(self, eng, tok, waits):
        if tok is None:
            return
        sem, val, src = tok
        if src == eng and (eng == "pe" or not self.same_engine_sync):
            return
        if self.seen[eng].get(sem, 0) >= val:
            return
        self.seen[eng][sem] = val
        waits.append((sem, val))

    def op(self, eng, fn, reads=(), writes=(), dma=False):
        waits = []
        for b in reads:
            self._need(eng, self.lastw.get(b), waits)
        for b in writes:
            self._need(eng, self.lastw.get(b), waits)
            for t in self.readers.get(b, {}).values():
                self._need(eng, t, waits)
        if dma:
            tok, prev = self._dma_token(eng)
            self._need(eng, prev, waits)
            inc = 16
        else:
            tok = self._engine_token(eng)
            inc = 1
        m = {}
        for s, v in waits:
            m[s] = max(m.get(s, 0), v)
        self.prog[eng].append((list(m.items()), fn, (tok[0], inc)))
        for b in reads:
            r = self.readers.setdefault(b, {})
            old = r.get(tok[0])
            if old is None or old[1] < tok[1]:
                r[tok[0]] = tok
        for b in writes:
            self.lastw[b] = tok
            self.readers[b] = {}
        return tok

    def final_wait(self, eng, bufs):
        waits = []
        for b in bufs:
            self._need(eng, self.lastw.get(b), waits)
        m = {}
        for s, v in waits:
            m[s] = max(m.get(s, 0), v)
        self.prog[eng].append((list(m.items()), None, None))

    def replay(self, eng, handle):
        for waits, fn, inc in self.prog[eng]:
            for s, v in waits:
                handle.wait_ge(self.sem_objs[s], v)
            if fn is not None:
                ins = fn(handle)
                ins.then_inc(self.sem_objs[inc[0]], inc[1])
        self.prog[eng] = []

    def emit(self):
        with self.nc.Block() as block:
            @block.tensor
            def _(e):
                self.replay("pe", e)

            @block.scalar
            def _(e):
                self.replay("act", e)

            @block.vector
            def _(e):
                self.replay("dve", e)

            @block.gpsimd
            def _(e):
                self.replay("pool", e)

            @block.sync
            def _(e):
                self.replay("sp", e)


class Ring:
    def __init__(self, names):
        self.names = list(names)
        self.i = 0

    def next(self):
        n = self.names[self.i % len(self.names)]
        self.i += 1
        return n


class _Stop(Exception):
    pass


_INS = []
_DBG = {}


def build_program(stop_after=None, dbg=False):
    holder = {}
    try:
        return _build(stop_after, dbg, holder)
    except _Stop:
        return holder["nc"]


def _build(stop_after, dbg, holder):
    nc = bass.Bass("TRN2", target_bir_lowering=False)
    holder["nc"] = nc

    def din(name, shape, dt=F32):
        _INS.append(name)
        return nc.dram_tensor(name, list(shape), dt, kind="ExternalInput").ap()

    def dint(name, shape, dt=F32):
        keep = dbg and (name in _DBG.get("keep", ()))
        return nc.dram_tensor(name, list(shape), dt, kind=("ExternalOutput" if keep else "Internal")).ap()

    def stage_end(k):
        if stop_after is not None and k >= stop_after:
            waits = []
            for q, pool in S.dma_pool.items():
                for sems, uses in zip(pool["sems"], pool["uses"]):
                    if uses > 0:
                        waits.append((sems[(uses - 1) // DMA_EPOCH], 16 * (((uses - 1) % DMA_EPOCH) + 1)))
            S.prog["sp"].append((waits, None, None))
            S.emit()
            raise _Stop()

    x_all = din("x_all", [8 * NT, D])
    x_own = din("x_own", [NT, D])
    x_halo = din("x_halo", [16, D])
    pos_all = din("pos_all", [1, 8 * NT], I32)
    pos_own = din("pos_own", [1, NT], I32)
    cst = din("cst", [P, 1024])
    masks = din("masks", [P, 2, 8, P])
    halov = din("halov", [P, 16])
    rmat = din("rmat", [P, P])
    c_in = din("c", [16, P])
    w_ada = din("w_ada", [D, 6 * D])
    b_ada = din("b_ada", [1, 6 * D])
    vecs = din("vecs", [96, P])
    final_g = din("final_g", [1, D])
    w_in = din("w_in", [D, 8 * D])
    lamv = din("lamv", [2, 2 * P])
    subln_g = din("subln_g", [1, 256])
    w_ao = din("w_attn_out", [D, D])
    w_co = din("w_conv_out", [D, D])
    w_o = din("w_o", [D, D])
    w_router = din("w_router", [D, NE])
    b_router = din("b_router", [1, NE])
    if stop_after is None or stop_after >= 5:
        w_up = din("w_up", [NE, D, 2 * D])
        b_up = din("b_up", [NE * 16, 256])
        w_down = din("w_down", [NE, D, D])
        b_down = din("b_down", [NE, D])
    out = nc.dram_tensor("out", [NT, D], F32, kind="ExternalOutput").ap()

    KT_all = dint("KT_all", [16, P, 8 * NT])
    V_all = dint("V_all", [8 * NT, D])
    QT = dint("QT", [16, P, NT])
    XT = dint("XT", [16, P, NT])
    YCP = dint("YCP", [16, P, NT])
    SGA = dint("SGA", [16, P, NT])
    SGC = dint("SGC", [16, P, NT])
    ONT = dint("ONT", [16, P, NT])
    X2TOK = dint("X2TOK", [NT, D])

    S = Sched(nc)
    es_glob = ExitStack()
    big = stop_after is None or stop_after >= 5

    uniq = [0]

    def sb(es, name, shape, dt=F32):
        uniq[0] += 1
        return es.enter_context(nc.sbuf_tensor(f"{name}_{uniq[0]}", list(shape), dt))

    def ps(es, name, shape, dt=F32):
        uniq[0] += 1
        return es.enter_context(nc.psum_tensor(f"{name}_{uniq[0]}", list(shape), dt))

    cstt = sb(es_glob, "cstt", [P, 1024])
    ident = cstt[:, 0:128]
    onesF = cstt[:, 128:256]
    tri = cstt[:, 256:384]
    iotaC = cstt[:, 384:640]
    invf = cstt[0:32, 640:641]
    vecT = sb(es_glob, "vecT", [P, 112])
    modT = sb(es_glob, "modT", [P, 96])
    AB = sb(es_glob, "AB", [P, 64])
    G2row = sb(es_glob, "G2row", [P, D])
    epst = sb(es_glob, "epst", [P, 1])
    neglam = sb(es_glob, "neglam", [P, 1])
    identb = sb(es_glob, "identb", [P, P], BF16)
    onesb = sb(es_glob, "onesb", [1, P], BF16)
    onesR = sb(es_glob, "onesR", [P, 2], F32R)
    rmt = sb(es_glob, "rmt", [P, P], F32R)

    S.op("sp", lambda e: e.dma_start(out=cstt[:], in_=cst), writes=["cst"], dma=True)
    S.op("pool", lambda e: e.dma_start(out=rmt[:], in_=rmat), writes=["rmt"], dma=True)
    S.op("dve", lambda e: e.memset(epst[:], EPS), writes=["eps"])
    S.op("dve", lambda e: e.tensor_copy(out=identb[:], in_=ident), reads=["cst"], writes=["identb"])
    S.op("dve", lambda e: e.tensor_copy(out=onesb[:], in_=cstt[0:1, 128:256]), reads=["cst"], writes=["onesb"])
    S.op("dve", lambda e: e.tensor_copy(out=onesR[:], in_=cstt[:, 128:130]), reads=["cst"], writes=["onesR"])

    with ExitStack() as es:
        rows = sb(es, "rows", [112, P])
        cact = sb(es, "cact", [P, 16])
        wsl = [sb(es, f"wsl{i}", [P, KC, 512]) for i in range(2)]
        brow = sb(es, "brow", [1, 6 * D])
        mrow = sb(es, "mrow", [1, 6 * D])
        lamt = sb(es, "lamt", [2, 2 * P])
        lamp = sb(es, "lamp", [2, P])
        lams = sb(es, "lams", [P, 4])
        pA = ps(es, "pA", [P, 512])
        pB = ps(es, "pB", [P, 512])
        pC = ps(es, "pC", [P, 512])
        S.op("sp", lambda e: e.dma_start(out=rows[0:16, :], in_=c_in), writes=["rows"], dma=True)
        S.op("sp", lambda e: e.dma_start(out=rows[16:112, :], in_=vecs), writes=["rows2"], dma=True)
        S.op("sp", lambda e: e.dma_start(out=brow[:], in_=b_ada), writes=["brow"], dma=True)
        S.op("sp", lambda e: e.dma_start(out=lamt[:], in_=lamv), writes=["lamt"], dma=True)
        S.op("pe", lambda e: e.transpose(pC[:, 0:112], rows[:], ident[0:112, 0:112]), reads=["rows", "rows2", "cst"], writes=["pC"])
        S.op("dve", lambda e: e.tensor_copy(out=vecT[:], in_=pC[:, 0:112]), reads=["pC"], writes=["vecT"])
        S.op("act", lambda e: e.activation(out=cact[:], in_=vecT[:, 0:16], func=AF.Silu), reads=["vecT"], writes=["cact"])
        wv = w_ada.rearrange("(c p) n -> p c n", p=P)
        prow = [pA, pB]
        for n in range(24):
            b = n % 2
            S.op("sp", lambda e, n=n, b=b: e.dma_start(out=wsl[b][:], in_=wv[:, :, n * 512:(n + 1) * 512]),
                 writes=[("wsl", b)], dma=True)
            for kc in range(KC):
                S.op("pe", lambda e, n=n, b=b, kc=kc: e.matmul(prow[b][0:1, :], cact[:, kc:kc + 1], wsl[b][:, kc, :], start=(kc == 0), stop=(kc == KC - 1)),
                     reads=[("wsl", b), "cact"], writes=[("prow", b)])
            S.op("dve", lambda e, n=n, b=b: e.tensor_tensor(out=mrow[:, n * 512:(n + 1) * 512], in0=prow[b][0:1, :], in1=brow[:, n * 512:(n + 1) * 512], op=ALU.add),
                 reads=[("prow", b), "brow"], writes=["mrow"])
        for j in range(96):
            S.op("pe", lambda e, j=j: e.matmul(pC[:, 128 + j:129 + j], mrow[0:1, j * P:(j + 1) * P], onesF[0:1, 0:1], start=True, stop=True),
                 reads=["mrow", "cst"], writes=["pC"])
        S.op("dve", lambda e: e.tensor_copy(out=modT[:], in_=pC[:, 128:224]), reads=["pC"], writes=["modT"])
        S.op("dve", lambda e: e.scalar_tensor_tensor(out=AB[:, 0:16], in0=modT[:, 16:32], scalar=1.0, in1=vecT[:, 16:32], op0=ALU.add, op1=ALU.mult),
             reads=["modT", "vecT"], writes=["AB"])
        S.op("dve", lambda e: e.tensor_copy(out=AB[:, 16:32], in_=modT[:, 0:16]), reads=["modT"], writes=["AB"])
        S.op("dve", lambda e: e.scalar_tensor_tensor(out=AB[:, 32:48], in0=modT[:, 64:80], scalar=1.0, in1=vecT[:, 32:48], op0=ALU.add, op1=ALU.mult),
             reads=["modT", "vecT"], writes=["AB"])
        S.op("dve", lambda e: e.tensor_copy(out=AB[:, 48:64], in_=modT[:, 48:64]), reads=["modT"], writes=["AB"])
        S.op("dve", lambda e: e.tensor_tensor(out=lamp[:], in0=lamt[:, 0:P], in1=lamt[:, P:2 * P], op=ALU.mult), reads=["lamt"], writes=["lamp"])
        S.op("pe", lambda e: e.transpose(pC[:, 260:262], lamp[0:2, :], ident[0:2, 0:2]), reads=["lamp", "cst"], writes=["pC"])
        S.op("dve", lambda e: e.tensor_copy(out=lams[:, 0:2], in_=pC[:, 260:262]), reads=["pC"], writes=["lams"])
        S.op("pe", lambda e: e.matmul(pC[:, 264:266], onesF[:, :], lams[:, 0:2], start=True, stop=True), reads=["lams", "cst"], writes=["pC"])
        S.op("act", lambda e: e.activation(out=lams[:, 2:4], in_=pC[:, 264:266], func=AF.Exp), reads=["pC"], writes=["lams"])
        S.op("dve", lambda e: e.tensor_tensor(out=neglam[:], in0=lams[:, 3:4], in1=lams[:, 2:3], op=ALU.subtract), reads=["lams"], writes=["neglam"])
        S.op("dve", lambda e: e.tensor_scalar(out=neglam[:], in0=neglam[:], scalar1=-LAMBDA_INIT, scalar2=None, op0=ALU.add), reads=["neglam"], writes=["neglam"])
        for q in range(4):
            S.op("pe", lambda e, q=q: e.matmul(pA[:], onesF[0:1, :], mrow[0:1, 5 * D + q * 512: 5 * D + (q + 1) * 512], start=True, stop=True),
                 reads=["mrow", "cst"], writes=[("prow", 0)])
            S.op("act", lambda e, q=q: e.activation(out=G2row[:, q * 512:(q + 1) * 512], in_=pA[:], func=AF.Copy), reads=[("prow", 0)], writes=["G2row"])
        if dbg:
            dbg0 = nc.dram_tensor("dbg0", [P, 96 + 64 + 1], F32, kind="ExternalOutput").ap()
            S.op("sp", lambda e: e.dma_start(out=dbg0[:, 0:96], in_=modT[:]), reads=["modT"], writes=["dbg0"], dma=True)
            S.op("sp", lambda e: e.dma_start(out=dbg0[:, 96:160], in_=AB[:]), reads=["AB"], writes=["dbg0"], dma=True)
            S.op("sp", lambda e: e.dma_start(out=dbg0[:, 160:161], in_=neglam[:], allow_slow_non_contiguous=True), reads=["neglam"], writes=["dbg0"], dma=True)
        S.emit()
        stage_end(0)

    A1, B1, A2, B2 = AB[:, 0:16], AB[:, 16:32], AB[:, 32:48], AB[:, 48:64]
    G1 = modT[:, 32:48]
    CW = vecT[:, 48:96]
    for mode in ("kv", "own"):
      with ExitStack() as es:
        own = (mode == "own")
        hT = sb(es, "hT", [P, KC, NT])
        hTr = hT[:].bitcast(F32R)
        xs = [sb(es, f"xs{i}", [P, D]) for i in range(2 if not own else 1)]
        if own:
            xs = [xs[0], xs[0]]
        xn = sb(es, "xn", [P, D])
        ssq = sb(es, "ssq", [P, 4])
        wsl = [sb(es, f"w1sl{i}", [P, KC, 256], F32R) for i in range(3)]
        ctab = sb(es, "ctab", [P, NT])
        stab = sb(es, "stab", [P, NT])
        ko = [sb(es, f"ko{i}", [P, 512]) for i in range(2)]
        kr = [sb(es, f"kr{i}", [P, 512], F32R) for i in range(2)]
        rtmp = sb(es, "rtmp", [P, 512])
        if own:
            hTh = sb(es, "hTh", [P, KC, 16])
            hThr = hTh[:].bitcast(F32R)
            xts = sb(es, "xts", [P, KC, P])
            ccT = sb(es, "ccT", [P, NT])
            YC = ccT
            ccTh = sb(es, "ccTh", [P, 16])
            U = sb(es, "U", [P, NS, 130])
            Cv = sb(es, "Cv", [P, NS, P])
            hvt = sb(es, "hvt", [P, 16])
            sg = [sb(es, f"sg{i}", [P, 512]) for i in range(2)]
            S.op("sp", lambda e: e.dma_start(out=hvt[:], in_=halov), writes=["hvt"], dma=True)
        else:
            vo = [sb(es, f"vo{i}", [P, 256]) for i in range(2)]
        pbank = [ps(es, f"pb{i}", [P, 512]) for i in range(8)]

        pring = Ring([0, 1, 2, 3])
        tring = Ring([4, 5])
        wring = Ring([0, 1, 2])
        koring = Ring([0, 1])
        voring = Ring([0, 1])
        sgring = Ring([0, 1])

        def rope_tables(pos_ap):
          with ExitStack() as es2:
            posi = sb(es2, "posi", [32, NT], I32)
            tt = [sb(es2, f"tt{i}", [32, NT]) for i in range(3)]
            tti = sb(es2, "tti", [32, NT], I32)
            S.op("sp", lambda e: e.dma_start(out=posi[:], in_=pos_ap.broadcast_to([32, NT])), writes=["posi"], dma=True)
            S.op("dve", lambda e: e.memset(ctab[:], 1.0), writes=["tabc"])
            S.op("dve", lambda e: e.memset(stab[:], 0.0), writes=["tabs"])
            S.op("dve", lambda e: e.tensor_copy(out=tt[0][:], in_=posi[:]), reads=["posi"], writes=["tt0"])
            for which, tab, off in ((("s", stab, 0.0), ("c", ctab, 0.25)) if "rope_a" not in _DBG.get("skip", ()) else ()):
                S.op("dve", lambda e, off=off: e.tensor_scalar(out=tt[1][:], in0=tt[0][:], scalar1=invf, scalar2=off, op0=ALU.mult, op1=ALU.add),
                     reads=["tt0", "cst"], writes=["tt1"])
                S.op("dve", lambda e: e.tensor_copy(out=tti[:], in_=tt[1][:]), reads=["tt1"], writes=["tti"])
                S.op("dve", lambda e: e.tensor_copy(out=tt[2][:], in_=tti[:]), reads=["tti"], writes=["tt2"])
                S.op("dve", lambda e: e.tensor_tensor(out=tt[1][:], in0=tt[1][:], in1=tt[2][:], op=ALU.subtract), reads=["tt1", "tt2"], writes=["tt1"])
                S.op("dve", lambda e: e.tensor_scalar(out=tt[2][:], in0=tt[1][:], scalar1=0.5, scalar2=None, op0=ALU.is_ge), reads=["tt1"], writes=["tt2"])
                S.op("dve", lambda e: e.tensor_tensor(out=tt[1][:], in0=tt[1][:], in1=tt[2][:], op=ALU.subtract), reads=["tt1", "tt2"], writes=["tt1"])
                S.op("dve", lambda e: e.tensor_scalar(out=tt[2][:], in0=tt[1][:], scalar1=-0.5, scalar2=None, op0=ALU.is_lt), reads=["tt1"], writes=["tt2"])
                S.op("dve", lambda e: e.tensor_tensor(out=tt[1][:], in0=tt[1][:], in1=tt[2][:], op=ALU.add), reads=["tt1", "tt2"], writes=["tt1"])
                if "rope_nosin" in _DBG.get("skip", ()):
                    S.op("dve", lambda e, tab=tab: e.tensor_copy(out=tab[0:32, :], in_=tt[1][:]), reads=["tt1"], writes=["tab" + which])
                else:
                    S.op("act", lambda e, tab=tab: e.activation(out=tab[0:32, :], in_=tt[1][:], func=AF.Sin, scale=2.0 * math.pi), reads=["tt1"], writes=["tab" + which])
            S.emit()

        def norm_slot(src_ap, np_, ncols, dstT, dst_name, col0, xbuf, raw_spill=None):
            if own:
                xbuf = 0
            xb = xs[xbuf]
            S.op("sp", lambda e: e.dma_start(out=xb[0:np_, :], in_=src_ap), writes=[("xs", xbuf)], dma=True)
            S.op("act", lambda e: e.activation(out=xn[0:np_, :], in_=xb[0:np_, :], func=AF.Square, accum_out=ssq[0:np_, 0:1]),
                 reads=[("xs", xbuf)], writes=["xn", "ssq"])
            S.op("act", lambda e: e.activation(out=ssq[0:np_, 1:2], in_=ssq[0:np_, 0:1], func=AF.Sqrt, bias=epst[0:np_, :], scale=1.0 / D),
                 reads=["ssq", "eps"], writes=["ssq1"])
            S.op("dve", lambda e: e.reciprocal(out=ssq[0:np_, 2:3], in_=ssq[0:np_, 1:2]), reads=["ssq1"], writes=["ssq2"])
            S.op("dve", lambda e: e.tensor_scalar(out=xn[0:np_, :], in0=xb[0:np_, :], scalar1=ssq[0:np_, 2:3], scalar2=None, op0=ALU.mult),
                 reads=[("xs", xbuf), "ssq2"], writes=["xn"])
            for g4 in range(4 if "norm_a" not in _DBG.get("skip", ()) else 0):
                pb = tring.next()
                for j in range(4):
                    kc = g4 * 4 + j
                    S.op("pe", lambda e, kc=kc, j=j, pb=pb: e.transpose(pbank[pb][:, j * P:j * P + np_], xn[0:np_, kc * P:(kc + 1) * P], ident[0:np_, 0:np_]),
                         reads=["xn", "cst"], writes=[("pb", pb)])
                for j in range(4):
                    kc = g4 * 4 + j
                    eng = "dve"
                    if eng == "dve":
                        S.op("dve", lambda e, kc=kc, j=j, pb=pb: e.tensor_scalar(out=dstT[:, kc, col0:col0 + np_], in0=pbank[pb][:, j * P:j * P + np_],
                                                                              scalar1=A1[:, kc:kc + 1], scalar2=B1[:, kc:kc + 1], op0=ALU.mult, op1=ALU.add),
                             reads=[("pb", pb), "AB"], writes=[(dst_name, kc)])
                    else:
                        S.op("act", lambda e, kc=kc, j=j, pb=pb: e.activation(out=dstT[:, kc, col0:col0 + np_], in_=pbank[pb][:, j * P:j * P + np_],
                                                                           func=AF.Identity, bias=B1[:, kc:kc + 1], scale=A1[:, kc:kc + 1]),
                             reads=[("pb", pb), "AB"], writes=[(dst_name, kc)])
            if raw_spill is not None:
                for g4 in range(4):
                    pb = tring.next()
                    for j in range(4):
                        kc = g4 * 4 + j
                        S.op("pe", lambda e, kc=kc, j=j, pb=pb: e.transpose(pbank[pb][:, j * P:(j + 1) * P], xb[:, kc * P:(kc + 1) * P], ident),
                             reads=[("xs", xbuf), "cst"], writes=[("pb", pb)])
                    S.op("act", lambda e, g4=g4, pb=pb: e.activation(out=xts[:, g4 * 4:(g4 + 1) * 4, :], in_=pbank[pb][:].rearrange("p (j t) -> p j t", j=4), func=AF.Copy),
                         reads=[("pb", pb)], writes=["xts"])
                S.op("sp", lambda e: e.dma_start(out=raw_spill, in_=xts[:]), reads=["xts"], writes=["XT"], dma=True)

        def load_slab(col0):
            wb = wring.next()
            S.op("pool", lambda e: e.dma_start(out=wsl[wb][:], in_=w_in[:, col0:col0 + 256].rearrange("(c p) n -> p c n", p=P)),
                 writes=[("w1sl", wb)], dma=True)
            return wb

        def mm_fm(wb, j, half, hname="hT"):
            pb = pring.next()
            for kc in range(KC):
                S.op("pe", lambda e, kc=kc, pb=pb: e.matmul(pbank[pb][:], wsl[wb][:, kc, j * P:(j + 1) * P], hTr[:, kc, half * 512:(half + 1) * 512],
                                                          start=(kc == 0), stop=(kc == KC - 1)),
                     reads=[("w1sl", wb), (hname, kc)], writes=[("pb", pb)])
            return pb

        def rope_evac(pb, dst_dram, tcol0):
            k = koring.next()
            S.op("act", lambda e: e.activation(out=ko[k][:], in_=pbank[pb][:], func=AF.Copy), reads=[("pb", pb)], writes=[("ko", k)])
            if "ropeev" in _DBG.get("skip", ()):
                S.op("sp", lambda e: e.dma_start(out=dst_dram, in_=ko[k][:]), reads=[("ko", k)], writes=["dram_qk"], dma=True)
                return
            S.op("act", lambda e: e.activation(out=kr[k][:], in_=pbank[pb][:], func=AF.Copy), reads=[("pb", pb)], writes=[("kr", k)])
            S.op("pe", lambda e: e.matmul(pbank[6][:], rmt[:], kr[k][:], start=True, stop=True), reads=[("kr", k), "rmt"], writes=[("pb", 6)])
            S.op("dve", lambda e: e.tensor_tensor(out=rtmp[:], in0=pbank[6][:], in1=stab[:, tcol0:tcol0 + 512], op=ALU.mult),
                 reads=[("pb", 6), "tabs"], writes=["rtmp"])
            S.op("dve", lambda e: e.tensor_tensor(out=ko[k][:], in0=ko[k][:], in1=ctab[:, tcol0:tcol0 + 512], op=ALU.mult),
                 reads=[("ko", k), "tabc"], writes=[("ko", k)])
            S.op("dve", lambda e: e.tensor_tensor(out=ko[k][:], in0=ko[k][:], in1=rtmp[:], op=ALU.add),
                 reads=[("ko", k), "rtmp"], writes=[("ko", k)])
            S.op("sp", lambda e: e.dma_start(out=dst_dram, in_=ko[k][:]), reads=[("ko", k)], writes=["dram_qk"], dma=True)

        for grp in (range(_DBG.get("ngrp", 8)) if not own else []):
            skip = _DBG.get("skip", ())
            if "rope" not in skip:
                rope_tables(pos_all[:, grp * NT:(grp + 1) * NT])
            for s in range(NS if "norm" not in skip else 0):
                norm_slot(x_all[grp * NT + s * P: grp * NT + (s + 1) * P, :], P, P, hTr, "hT", s * P, s % 2)
            for sl in range(8 if "k" not in skip else 0):
                wb = load_slab(D + sl * 256)
                for j in range(2 if "k_nomm" not in skip else 0):
                    ct = sl * 2 + j
                    for half in range(2):
                        pb = mm_fm(wb, j, half)
                        rope_evac(pb, KT_all[ct, :, grp * NT + half * 512: grp * NT + (half + 1) * 512], half * 512)
            for sl in range(8 if "v" not in skip else 0):
                wb = load_slab(2 * D + sl * 256)
                for s in range(NS):
                    pb = pring.next()
                    for kc in range(KC):
                        S.op("pe", lambda e, kc=kc, pb=pb, s=s, wb=wb: e.matmul(pbank[pb][:, 0:256], hTr[:, kc, s * P:(s + 1) * P], wsl[wb][:, kc, :],
                                                                            start=(kc == 0), stop=(kc == KC - 1)),
                             reads=[("w1sl", wb), ("hT", kc)], writes=[("pb", pb)])
                    v = voring.next()
                    S.op("act", lambda e, v=v, pb=pb: e.activation(out=vo[v][:], in_=pbank[pb][:, 0:256], func=AF.Copy),
                         reads=[("pb", pb)], writes=[("vo", v)])
                    S.op("sp", lambda e, v=v, s=s, sl=sl, grp=grp: e.dma_start(out=V_all[grp * NT + s * P: grp * NT + (s + 1) * P, sl * 256:(sl + 1) * 256], in_=vo[v][:]),
                         reads=[("vo", v)], writes=["dram_v"], dma=True)
            S.emit()
        if not own:
            stage_end(1)
            continue

        rope_tables(pos_own)
        for s in range(NS):
            norm_slot(x_own[s * P:(s + 1) * P, :], P, P, hTr, "hT", s * P, s % 2,
                      raw_spill=XT[:, :, s * P:(s + 1) * P].rearrange("c p t -> p c t"))
        norm_slot(x_halo, 16, 16, hThr, "hTh", 0, 0)
        for sl in range(8):
            wb = load_slab(sl * 256)
            for j in range(2):
                ct = sl * 2 + j
                for half in range(2):
                    pb = mm_fm(wb, j, half)
                    rope_evac(pb, QT[ct, :, half * 512:(half + 1) * 512], half * 512)
        for sl in range(8):
            wcc = load_slab(4 * D + sl * 256)
            wch = load_slab(5 * D + sl * 256)
            wcb = load_slab(3 * D + sl * 256)
            for j in range(2):
                ct = sl * 2 + j
                for half in range(2):
                    pb = mm_fm(wcc, j, half)
                    S.op("act", lambda e, pb=pb, half=half: e.activation(out=ccT[:, half * 512:(half + 1) * 512], in_=pbank[pb][:], func=AF.Copy),
                         reads=[("pb", pb)], writes=["ccT"])
                pb = pring.next()
                for kc in range(KC):
                    S.op("pe", lambda e, kc=kc, pb=pb, j=j, wcc=wcc: e.matmul(pbank[pb][:, 0:16], wsl[wcc][:, kc, j * P:(j + 1) * P], hThr[:, kc, :], start=(kc == 0), stop=(kc == KC - 1)),
                         reads=[("w1sl", wcc), ("hTh", kc)], writes=[("pb", pb)])
                S.op("act", lambda e, pb=pb: e.activation(out=ccTh[:], in_=pbank[pb][:, 0:16], func=AF.Copy), reads=[("pb", pb)], writes=["ccTh"])
                for half in range(2):
                    pb = mm_fm(wch, j, half)
                    S.op("dve", lambda e, pb=pb, half=half: e.tensor_tensor(out=U[:, half * 4:(half + 1) * 4, 2:130],
                                                                         in0=pbank[pb][:].rearrange("p (s t) -> p s t", s=4),
                                                                         in1=ccT[:, half * 512:(half + 1) * 512].rearrange("p (s t) -> p s t", s=4), op=ALU.mult),
                         reads=[("pb", pb), "ccT"], writes=["U"])
                pb = pring.next()
                for kc in range(KC):
                    S.op("pe", lambda e, kc=kc, pb=pb, j=j, wch=wch: e.matmul(pbank[pb][:, 0:16], wsl[wch][:, kc, j * P:(j + 1) * P], hThr[:, kc, :], start=(kc == 0), stop=(kc == KC - 1)),
                         reads=[("w1sl", wch), ("hTh", kc)], writes=[("pb", pb)])
                S.op("dve", lambda e, pb=pb: e.tensor_tensor(out=ccTh[:], in0=pbank[pb][:, 0:16], in1=ccTh[:], op=ALU.mult), reads=[("pb", pb), "ccTh"], writes=["ccTh"])
                S.op("dve", lambda e: e.tensor_tensor(out=U[:, :, 0:2], in0=ccTh[:].rearrange("p (s t) -> p s t", s=8), in1=hvt[:].rearrange("p (s t) -> p s t", s=8), op=ALU.mult),
                     reads=["ccTh", "hvt"], writes=["U"])
                S.op("dve", lambda e, ct=ct: e.tensor_scalar(out=Cv[:], in0=U[:, :, 2:130], scalar1=CW[:, 32 + ct:33 + ct], scalar2=None, op0=ALU.mult),
                     reads=["U", "vecT"], writes=["Cv"])
                S.op("dve", lambda e, ct=ct: e.scalar_tensor_tensor(out=Cv[:], in0=U[:, :, 1:129], scalar=CW[:, 16 + ct:17 + ct], in1=Cv[:], op0=ALU.mult, op1=ALU.add),
                     reads=["U", "vecT", "Cv"], writes=["Cv"])
                S.op("dve", lambda e, ct=ct: e.scalar_tensor_tensor(out=Cv[:], in0=U[:, :, 0:128], scalar=CW[:, ct:ct + 1], in1=Cv[:], op0=ALU.mult, op1=ALU.add),
                     reads=["U", "vecT", "Cv"], writes=["Cv"])
                for half in range(2):
                    pb = mm_fm(wcb, j, half)
                    S.op("dve", lambda e, pb=pb, half=half: e.tensor_tensor(out=YC[:, half * 512:(half + 1) * 512], in0=pbank[pb][:],
                                                                         in1=Cv[:, half * 4:(half + 1) * 4, :].rearrange("p s t -> p (s t)"), op=ALU.mult),
                         reads=[("pb", pb), "Cv"], writes=["ccT"])
                S.op("sp", lambda e, ct=ct: e.dma_start(out=YCP[ct], in_=YC[:]), reads=["ccT"], writes=["dram_ycp"], dma=True)
        for gi, (gbase, gdst) in enumerate(((6 * D, SGA), (7 * D, SGC))):
            for sl in range(8):
                wb = load_slab(gbase + sl * 256)
                for j in range(2):
                    ct = sl * 2 + j
                    for half in range(2):
                        pb = mm_fm(wb, j, half)
                        g = sgring.next()
                        S.op("act", lambda e, pb=pb, g=g: e.activation(out=sg[g][:], in_=pbank[pb][:], func=AF.Sigmoid), reads=[("pb", pb)], writes=[("sg", g)])
                        S.op("sp", lambda e, g=g, ct=ct, half=half, gdst=gdst: e.dma_start(out=gdst[ct, :, half * 512:(half + 1) * 512], in_=sg[g][:]),
                             reads=[("sg", g)], writes=["dram_sg"], dma=True)
        S.emit()
        stage_end(2)

    with ExitStack() as es:
        Kb0 = sb(es, "Kb0", [P, 8 * NT], F32R)
        Kb = [Kb0, Kb0]
        Vb = sb(es, "Vb", [P, 64, 256], F32R)
        Qb0 = sb(es, "Qb0", [P, NT], F32R)
        Qb = [Qb0, Qb0]
        PT = [sb(es, f"PT{i}", [P, 512]) for i in range(3)]
        mk = sb(es, "mk", [P, 2, 8, P])
        Oc = [sb(es, f"Oc{i}", [P, NS, 256]) for i in range(2)]
        lc = sb(es, "lc", [P, 2, NS])
        rl = sb(es, "rl", [P, 2, NS])
        osl = sb(es, "osl", [P, 256])
        osq = sb(es, "osq", [P, 256])
        oss = sb(es, "oss", [P, 4])
        grow = sb(es, "grow", [P, 256])
        onT = sb(es, "onT", [P, 2, NT])
        pO = [ps(es, f"pO{i}", [P, 2, 256]) for i in range(4)]
        pL = ps(es, "pL", [P, 512])
        pS = [ps(es, f"pS{i}", [P, 512]) for i in range(2)]
        pT = ps(es, "pT", [P, 512])
        S.op("sp", lambda e: e.dma_start(out=mk[:], in_=masks), writes=["mk"], dma=True)
        S.op("sp", lambda e: e.dma_start(out=grow[:], in_=subln_g.broadcast_to([P, 256])), writes=["grow"], dma=True)
        S.op("dve", lambda e: e.tensor_scalar(out=grow[:], in0=grow[:], scalar1=1.0 - LAMBDA_INIT, scalar2=None, op0=ALU.mult), reads=["grow"], writes=["grow"])
        sring = Ring([0, 1])
        ptring = Ring([0, 1, 2])
        Vv = V_all.rearrange("(b p) n -> p b n", p=P)
        for h in range(8):
            for q4 in range(4):
                S.op("pool", lambda e, h=h, q4=q4: e.dma_start(out=Vb[:, q4 * 16:(q4 + 1) * 16, :], in_=Vv[:, q4 * 16:(q4 + 1) * 16, h * 256:(h + 1) * 256]),
                     writes=["Vb"], dma=True)
            for c in range(2):
                comp = 2 * h + c
                kb_ = 0
                for q2 in range(2):
                    S.op("pool", lambda e, comp=comp, kb_=kb_, q2=q2: e.dma_start(out=Kb[kb_][:, q2 * 4096:(q2 + 1) * 4096], in_=KT_all[comp, :, q2 * 4096:(q2 + 1) * 4096]),
                         writes=[("Kb", kb_)], dma=True)
                S.op("pool", lambda e, comp=comp, kb_=kb_: e.dma_start(out=Qb[kb_][:], in_=QT[comp]), writes=[("Qb", kb_)], dma=True)
                for ip in range(NS):
                    for r in range(8):
                        kblk = r * 8 + ip
                        slots = list(range(ip, NS))
                        n = len(slots)
                        chunks = [slots] if n <= 4 else [slots[:(n + 1) // 2], slots[(n + 1) // 2:]]
                        for ch in chunks:
                            ncol = len(ch) * P
                            c0 = ch[0] * P
                            sp_ = sring.next()
                            S.op("pe", lambda e, sp_=sp_, kblk=kblk, kb_=kb_, c0=c0, ncol=ncol: e.matmul(pS[sp_][:, 0:ncol], Kb[kb_][:, kblk * P:(kblk + 1) * P], Qb[kb_][:, c0:c0 + ncol], start=True, stop=True),
                                 reads=[("Kb", kb_), ("Qb", kb_)], writes=[("pS", sp_)])
                            pt_ = ptring.next()
                            S.op("act", lambda e, sp_=sp_, pt_=pt_, ncol=ncol: e.activation(out=PT[pt_][:, 0:ncol].bitcast(F32R), in_=pS[sp_][:, 0:ncol], func=AF.Exp, scale=SCALE),
                                 reads=[("pS", sp_)], writes=[("PT", pt_)])
                            if ch[0] == ip:
                                S.op("dve", lambda e, pt_=pt_, ip=ip, r=r: e.tensor_tensor(out=PT[pt_][:, 0:P].bitcast(F32R), in0=PT[pt_][:, 0:P], in1=mk[:, ip % 2, r, :], op=ALU.mult),
                                     reads=[("PT", pt_), "mk"], writes=[("PT", pt_)])
                            for si, sl_ in enumerate(ch):
                                first = (ip == 0 and r == 0)
                                last = (ip == sl_ and r == 7)
                                S.op("pe", lambda e, pt_=pt_, si=si, sl_=sl_, kblk=kblk, first=first, last=last: e.matmul(pO[sl_ // 2][:, sl_ % 2, :], PT[pt_][:, si * P:(si + 1) * P].bitcast(F32R), Vb[:, kblk, :], start=(first and sl_ % 2 == 0), stop=last),
                                     reads=[("PT", pt_), "Vb"], writes=[("pO", sl_)])
                                S.op("pe", lambda e, pt_=pt_, si=si, sl_=sl_, first=first, last=last: e.matmul(pL[:, 2 * sl_:2 * sl_ + 2], PT[pt_][:, si * P:(si + 1) * P].bitcast(F32R), onesR[:, 0:2], start=(first and sl_ == 0), stop=last),
                                     reads=[("PT", pt_), "onesR"], writes=[("pL", sl_)])
                for s4 in range(4):
                    S.op("act" if s4 % 2 == 0 else "dve",
                         (lambda e, s4=s4, c=c: e.activation(out=Oc[c][:, 2 * s4:2 * s4 + 2, :], in_=pO[s4][:], func=AF.Copy)) if s4 % 2 == 0 else
                         (lambda e, s4=s4, c=c: e.tensor_scalar(out=Oc[c][:, 2 * s4:2 * s4 + 2, :], in0=pO[s4][:], scalar1=1.0, scalar2=None, op0=ALU.mult)),
                         reads=[("pO", 2 * s4), ("pO", 2 * s4 + 1)], writes=[("Oc", c)])
                S.op("dve", lambda e, c=c: e.tensor_scalar(out=lc[:, c, :], in0=pL[:, 0:2 * NS].rearrange("p (s two) -> p s two", two=2)[:, :, 0], scalar1=1.0, scalar2=None, op0=ALU.mult), reads=[("pL", s) for s in range(NS)], writes=[("lc", c)])
            S.op("dve", lambda e: e.reciprocal(out=rl[:], in_=lc[:]), reads=[("lc", 0), ("lc", 1)], writes=["rl"])
            S.op("dve", lambda e: e.tensor_scalar(out=rl[:, 1, :], in0=rl[:, 1, :], scalar1=neglam[:, 0:1], scalar2=None, op0=ALU.mult), reads=["rl", "neglam"], writes=["rl"])
            for s in range(NS):
                S.op("dve", lambda e, s=s: e.tensor_scalar(out=osl[:], in0=Oc[0][:, s, :], scalar1=rl[:, 0, s:s + 1], scalar2=None, op0=ALU.mult),
                     reads=[("Oc", 0), "rl"], writes=["osl"])
                S.op("dve", lambda e, s=s: e.scalar_tensor_tensor(out=osl[:], in0=Oc[1][:, s, :], scalar=rl[:, 1, s:s + 1], in1=osl[:], op0=ALU.mult, op1=ALU.add),
                     reads=[("Oc", 1), "rl", "osl"], writes=["osl"])
                S.op("act", lambda e: e.activation(out=osq[:], in_=osl[:], func=AF.Square, accum_out=oss[:, 0:1]), reads=["osl"], writes=["osq", "oss"])
                S.op("act", lambda e: e.activation(out=oss[:, 1:2], in_=oss[:, 0:1], func=AF.Sqrt, bias=epst[:], scale=1.0 / 256), reads=["oss", "eps"], writes=["oss1"])
                S.op("dve", lambda e: e.reciprocal(out=oss[:, 2:3], in_=oss[:, 1:2]), reads=["oss1"], writes=["oss2"])
                S.op("dve", lambda e: e.scalar_tensor_tensor(out=osq[:], in0=osl[:], scalar=oss[:, 2:3], in1=grow[:], op0=ALU.mult, op1=ALU.mult),
                     reads=["osl", "oss2", "grow"], writes=["osq"])
                for j in range(2):
                    S.op("pe", lambda e, j=j: e.transpose(pT[:, j * P:(j + 1) * P], osq[:, j * P:(j + 1) * P], ident), reads=["osq", "cst"], writes=["pT"])
                S.op("act", lambda e, s=s: e.activation(out=onT[:, :, s * P:(s + 1) * P], in_=pT[:, 0:256].rearrange("p (j t) -> p j t", j=2), func=AF.Copy),
                     reads=["pT"], writes=["onT"])
            S.op("sp", lambda e, h=h: e.dma_start(out=ONT[2 * h:2 * h + 2].rearrange("c p t -> p c t"), in_=onT[:]), reads=["onT"], writes=["dram_ont"], dma=True)
        S.emit()
        stage_end(3)

    X2T = dint("X2T", [16, P, NT])
    H2T = dint("H2T", [16, P, NT], BF16)
    es_moe = ExitStack()
    Wall = sb(es_moe, "Wall", [P, NS, NE])
    msk = sb(es_moe, "msk", [P, NS, NE])
    HT = 512
    for hf in range(2):
      t0 = hf * HT
      with ExitStack() as es_x:
        x2T = sb(es_x, "x2T", [P, KC, HT])
        with ExitStack() as es_m:
            mR = sb(es_m, "mR", [P, KC, HT], F32R)

            def gemm(es, Wd, inr, inname, evac):
                wsl = [sb(es, f"w3sl{i}", [P, KC, P], F32R) for i in range(2)]
                pbank = [ps(es, f"p3b{i}", [P, 512]) for i in range(4)]
                for ct in range(16):
                    wb = ct % 2
                    S.op("pool", lambda e, wb=wb, ct=ct: e.dma_start(out=wsl[wb][:], in_=Wd[:, ct * P:(ct + 1) * P].rearrange("(c p) n -> p c n", p=P)),
                         writes=[("w3sl", wb)], dma=True)
                    pb = ct % 4
                    for kc in range(KC):
                        S.op("pe", lambda e, kc=kc, pb=pb, wb=wb: e.matmul(pbank[pb][:], wsl[wb][:, kc, :], inr[:, kc, :], start=(kc == 0), stop=(kc == KC - 1)),
                             reads=[("w3sl", wb), (inname, kc)], writes=[("p3b", pb)])
                    evac(ct, pbank[pb], ("p3b", pb))

            with ExitStack() as es:
                bufIn = sb(es, "bufIn", [P, KC, HT], F32R)
                gt = [sb(es, f"gt{i}", [P, HT]) for i in range(4)]
                tmp = [sb(es, f"tmp3{i}", [P, HT]) for i in range(2)]
                gring = Ring([0, 1, 2, 3])
                tring = Ring([0, 1])

                def load_act(src):
                    for q in range(4):
                        S.op("pool", lambda e, q=q, src=src: e.dma_start(out=bufIn[:, q * 4:(q + 1) * 4, :], in_=src[q * 4:(q + 1) * 4, :, t0:t0 + HT].rearrange("c p t -> p c t")),
                             writes=[("bufIn", k) for k in range(q * 4, q * 4 + 4)], dma=True)

                def ev_yc(ct, pbt, pbn):
                    g = gring.next()
                    t = tring.next()
                    S.op("sp", lambda e: e.dma_start(out=gt[g][:], in_=SGC[ct, :, t0:t0 + HT]), reads=[("SGC", ct)], writes=[("gt", g)], dma=True)
                    S.op("dve", lambda e: e.tensor_tensor(out=tmp[t][:], in0=pbt[:], in1=gt[g][:], op=ALU.mult), reads=[pbn, ("gt", g)], writes=[("tmp3", t)])
                    S.op("sp", lambda e: e.dma_start(out=SGC[ct, :, t0:t0 + HT], in_=tmp[t][:]), reads=[("tmp3", t)], writes=[("SGC", ct)], dma=True)

                def ev_ya(ct, pbt, pbn):
                    g = gring.next()
                    g2 = gring.next()
                    t = tring.next()
                    S.op("sp", lambda e: e.dma_start(out=gt[g][:], in_=SGA[ct, :, t0:t0 + HT]), writes=[("gt", g)], dma=True)
                    S.op("sp", lambda e: e.dma_start(out=gt[g2][:], in_=SGC[ct, :, t0:t0 + HT]), reads=[("SGC", ct)], writes=[("gt", g2)], dma=True)
                    S.op("dve", lambda e: e.tensor_tensor(out=tmp[t][:], in0=pbt[:], in1=gt[g][:], op=ALU.mult), reads=[pbn, ("gt", g)], writes=[("tmp3", t)])
                    S.op("dve", lambda e: e.tensor_tensor(out=mR[:, ct, :], in0=tmp[t][:], in1=gt[g2][:], op=ALU.add), reads=[("tmp3", t), ("gt", g2)], writes=[("mR", ct)])

                load_act(YCP)
                with ExitStack() as esg:
                    if "s3_0" not in _DBG.get("skip", ()):
                        gemm(esg, w_co, bufIn, "bufIn", ev_yc)
                    load_act(ONT)
                    S.emit()
                    if "s3_1" in _DBG.get("skip", ()) or ("s3_1b" in _DBG.get("skip", ()) and hf == 1):
                        stage_end(4)
                with ExitStack() as esg:
                    gemm(esg, w_ao, bufIn, "bufIn", ev_ya)
                    S.emit()
                    if "s3_2" in _DBG.get("skip", ()):
                        stage_end(4)
            with ExitStack() as es:
                gt = [sb(es, f"gto{i}", [P, HT]) for i in range(2)]
                gring = Ring([0, 1])

                def ev_o(ct, pbt, pbn):
                    g = gring.next()
                    S.op("sp", lambda e: e.dma_start(out=gt[g][:], in_=XT[ct, :, t0:t0 + HT]), writes=[("gto", g)], dma=True)
                    S.op("dve", lambda e: e.scalar_tensor_tensor(out=x2T[:, ct, :], in0=pbt[:], scalar=G1[:, ct:ct + 1], in1=gt[g][:], op0=ALU.mult, op1=ALU.add),
                         reads=[pbn, ("gto", g), "modT"], writes=[("x2T", ct)])
                gemm(es, w_o, mR, "mR", ev_o)
                S.emit()
                if "s3_3" in _DBG.get("skip", ()) or ("s3_3b" in _DBG.get("skip", ()) and hf == 1):
                    stage_end(4)
        if "s3b" in _DBG.get("skip", ()):
            continue
        with ExitStack() as es:
            h2T = sb(es, "h2T", [P, KC, HT])
            sqt = [sb(es, f"sqt{i}", [P, HT], F32R) for i in range(2)]
            rstd2 = sb(es, "rstd2", [P, HT])
            onesRR = sb(es, "onesRR", [P, P], F32R)
            wr = sb(es, "wr", [P, KC, NE])
            brt = sb(es, "brt", [1, NE])
            lg = sb(es, "lg", [P, NE])
            mx8 = sb(es, "mx8", [P, 8])
            sm = sb(es, "sm", [P, 4])
            ex = sb(es, "ex", [P, NE])
            h2b = sb(es, "h2b", [P, KC, HT], BF16)
            pbank = [ps(es, f"p4b{i}", [P, 512]) for i in range(8)]
            sqring = Ring([0, 1])
            for q in range(4):
                S.op("sp", lambda e, q=q: e.dma_start(out=X2T[q * 4:(q + 1) * 4, :, t0:t0 + HT].rearrange("c p t -> p c t"), in_=x2T[:, q * 4:(q + 1) * 4, :]),
                     reads=[("x2T", k) for k in range(q * 4, q * 4 + 4)], writes=["dram_x2"], dma=True)
            S.op("dve", lambda e: e.tensor_copy(out=onesRR[:], in_=onesF), reads=["cst"], writes=["onesRR"])
            for kc in range(KC):
                q = sqring.next()
                S.op("act", lambda e, q=q, kc=kc: e.activation(out=sqt[q][:], in_=x2T[:, kc, :], func=AF.Square), reads=[("x2T", kc)], writes=[("sqt", q)])
                S.op("pe", lambda e, q=q, kc=kc: e.matmul(pbank[4][:], onesRR[:], sqt[q][:], start=(kc == 0), stop=(kc == KC - 1)),
                     reads=[("sqt", q), "onesRR"], writes=[("p4b", 4)])
            S.op("act", lambda e: e.activation(out=rstd2[:], in_=pbank[4][:], func=AF.Sqrt, bias=epst[:], scale=1.0 / D), reads=[("p4b", 4), "eps"], writes=["rstd2"])
            S.op("dve", lambda e: e.reciprocal(out=rstd2[:], in_=rstd2[:]), reads=["rstd2"], writes=["rstd2"])
            for kc in range(KC):
                S.op("dve", lambda e, kc=kc: e.tensor_tensor(out=h2T[:, kc, :], in0=x2T[:, kc, :], in1=rstd2[:], op=ALU.mult),
                     reads=[("x2T", kc), "rstd2"], writes=[("h2T", kc)])
                S.op("dve", lambda e, kc=kc: e.tensor_scalar(out=h2T[:, kc, :], in0=h2T[:, kc, :], scalar1=A2[:, kc:kc + 1], scalar2=B2[:, kc:kc + 1], op0=ALU.mult, op1=ALU.add),
                     reads=[("h2T", kc), "AB"], writes=[("h2T", kc)])
            S.op("sp", lambda e: e.dma_start(out=wr[:], in_=w_router.rearrange("(c p) n -> p c n", p=P)), writes=["wr"], dma=True)
            S.op("sp", lambda e: e.dma_start(out=brt[:], in_=b_router), writes=["brt"], dma=True)
            for sl_ in range(4):
                s = hf * 4 + sl_
                for kc in range(KC):
                    S.op("pe", lambda e, sl_=sl_, kc=kc: e.matmul(pbank[6][:, 0:NE], h2T[:, kc, sl_ * P:(sl_ + 1) * P], wr[:, kc, :], start=(kc == 0), stop=False),
                         reads=[("h2T", kc), "wr"], writes=[("p4b", 6)])
                S.op("pe", lambda e: e.matmul(pbank[6][:, 0:NE], onesF[0:1, :], brt[:], start=False, stop=True), reads=["brt", "cst"], writes=[("p4b", 6)])
                S.op("dve", lambda e: e.tensor_scalar(out=lg[:], in0=pbank[6][:, 0:NE], scalar1=1.0, scalar2=None, op0=ALU.mult), reads=[("p4b", 6)], writes=["lg"])
                S.op("dve", lambda e: e.max(out=mx8[:], in_=lg[:]), reads=["lg"], writes=["mx8"])
                S.op("dve", lambda e, s=s: e.tensor_scalar(out=msk[:, s, :], in0=lg[:], scalar1=mx8[:, 3:4], scalar2=None, op0=ALU.is_ge), reads=["lg", "mx8"], writes=["msk"])
                S.op("dve", lambda e: e.tensor_scalar(out=sm[:, 0:1], in0=mx8[:, 0:1], scalar1=-1.0, scalar2=None, op0=ALU.mult), reads=["mx8"], writes=["sm"])
                S.op("act", lambda e: e.activation(out=ex[:], in_=lg[:], func=AF.Exp, bias=sm[:, 0:1], scale=1.0), reads=["lg", "sm"], writes=["ex"])
                S.op("dve", lambda e, s=s: e.tensor_tensor(out=ex[:], in0=ex[:], in1=msk[:, s, :], op=ALU.mult), reads=["ex", "msk"], writes=["ex"])
                S.op("dve", lambda e: e.tensor_reduce(out=sm[:, 1:2], in_=ex[:], axis=mybir.AxisListType.X, op=ALU.add), reads=["ex"], writes=["sm1"])
                S.op("dve", lambda e: e.reciprocal(out=sm[:, 2:3], in_=sm[:, 1:2]), reads=["sm1"], writes=["sm2"])
                S.op("dve", lambda e, s=s: e.tensor_scalar(out=Wall[:, s, :], in0=ex[:], scalar1=sm[:, 2:3], scalar2=None, op0=ALU.mult), reads=["ex", "sm2"], writes=["Wall"])
            for kc in range(KC):
                if kc % 2 == 0:
                    S.op("act", lambda e, kc=kc: e.activation(out=h2b[:, kc, :], in_=h2T[:, kc, :], func=AF.Copy), reads=[("h2T", kc)], writes=[("h2b", kc)])
                else:
                    S.op("dve", lambda e, kc=kc: e.tensor_scalar(out=h2b[:, kc, :], in0=h2T[:, kc, :], scalar1=1.0, scalar2=None, op0=ALU.mult), reads=[("h2T", kc)], writes=[("h2b", kc)])
            for q in range(4):
                S.op("sp", lambda e, q=q: e.dma_start(out=H2T[q * 4:(q + 1) * 4, :, t0:t0 + HT].rearrange("c p t -> p c t"), in_=h2b[:, q * 4:(q + 1) * 4, :]),
                     reads=[("h2b", k) for k in range(q * 4, q * 4 + 4)], writes=["dram_h2"], dma=True)
            S.emit()
            if "s3_h0" in _DBG.get("skip", ()):
                stage_end(4)
    if dbg:
        dbg4 = nc.dram_tensor("dbg4", [P, NS * NE], F32, kind="ExternalOutput").ap()
        S.op("sp", lambda e: e.dma_start(out=dbg4, in_=Wall[:].rearrange("p s e -> p (s e)")), reads=["Wall"], writes=["dbg4"], dma=True)
        S.emit()
    stage_end(4)
    h2m = sb(es_moe, "h2m", [P, KC, NT], BF16)
    for q in range(4):
        S.op("sp", lambda e, q=q: e.dma_start(out=h2m[:, q * 4:(q + 1) * 4, :], in_=H2T[q * 4:(q + 1) * 4].rearrange("c p t -> p c t")),
             reads=["dram_h2"], writes=["h2m"], dma=True)

    with ExitStack() as es_a:
        acc = sb(es_a, "acc", [P, NS, D])
        with ExitStack() as es:
            actT = sb(es, "actT", [P, KC, NT], BF16)
            wu = [sb(es, f"wu{i}", [P, KC, 256], BF16) for i in range(2)]
            wd = [sb(es, f"wd{i}", [P, KC, 256], BF16) for i in range(2)]
            bdr = sb(es, "bdr", [1, D], BF16)
            BU = sb(es, "BU", [P, 256])
            buT = sb(es, "buT", [P, 2, NE * 16])
            gtt = [sb(es, f"gtt{i}", [P, 512]) for i in range(2)]
            stt = [sb(es, f"stt{i}", [P, 512]) for i in range(2)]
            ltt = [sb(es, f"ltt{i}", [P, 512]) for i in range(2)]
            pG = [ps(es, f"pG{i}", [P, 512]) for i in range(2)]
            pLn = [ps(es, f"pLn{i}", [P, 512]) for i in range(2)]
            pY = [ps(es, f"pY{i}", [P, 512]) for i in range(3)]
            pM = ps(es, "pM", [P, 512])
            uring, yring, ering = Ring([0, 1]), Ring([0, 1, 2]), Ring([0, 1])
            wuring, wdring = Ring([0, 1]), Ring([0, 1])
            for g in range(4):
                S.op("sp", lambda e, g=g: e.dma_start(out=BU[:], in_=b_up[g * P:(g + 1) * P, :]), writes=["BU"], dma=True)
                for par in range(2):
                    S.op("pe", lambda e, par=par: e.transpose(pM[:, par * P:(par + 1) * P], BU[:].rearrange("p (m two) -> p m two", two=2)[:, :, par], ident),
                         reads=["BU", "cst"], writes=["pM"])
                S.op("dve", lambda e, g=g: e.tensor_scalar(out=buT[:, :, g * P:(g + 1) * P], in0=pM[:, 0:256].rearrange("p (two m) -> p two m", two=2), scalar1=1.0, scalar2=None, op0=ALU.mult),
                     reads=["pM"], writes=["buT"])
            S.op("dve", lambda e: e.tensor_scalar(out=buT[:, 1, :], in0=buT[:, 1, :], scalar1=1.0, scalar2=None, op0=ALU.add), reads=["buT"], writes=["buT"])
            for ex_ in range(NE):
                for j in range(16):
                    wb = wuring.next()
                    S.op("pool", lambda e, wb=wb, j=j, ex_=ex_: e.dma_start(out=wu[wb][:], in_=w_up[ex_, :, j * 256:(j + 1) * 256].rearrange("(c p) n -> p c n", p=P)),
                         writes=[("wu", wb)], dma=True)
                    col = ex_ * 16 + j
                    for half in range(2):
                        pu = uring.next()
                        for par, pt in ((0, pG[pu]), (1, pLn[pu])):
                            for kc in range(KC):
                                S.op("pe", lambda e, wb=wb, kc=kc, par=par, pt=pt, half=half: e.matmul(pt[:], wu[wb][:, kc, :].rearrange("p (m two) -> p m two", two=2)[:, :, par], h2m[:, kc, half * 512:(half + 1) * 512],
                                                                                                start=(kc == 0), stop=(kc == KC - 1)),
                                     reads=[("wu", wb), "h2m"], writes=[("pGL", pu, par)])
                        q = ering.next()
                        S.op("dve", lambda e, pu=pu, q=q, col=col: e.tensor_scalar(out=gtt[q][:], in0=pG[pu][:], scalar1=buT[:, 0, col:col + 1], scalar2=7.0, op0=ALU.add, op1=ALU.min),
                             reads=[("pGL", pu, 0), "buT"], writes=[("gtt", q)])
                        S.op("act", lambda e, q=q: e.activation(out=stt[q][:], in_=gtt[q][:], func=AF.Sigmoid, scale=1.702), reads=[("gtt", q)], writes=[("stt", q)])
                        S.op("dve", lambda e, pu=pu, q=q, col=col: e.tensor_scalar(out=ltt[q][:], in0=pLn[pu][:], scalar1=buT[:, 1, col:col + 1], scalar2=8.0, op0=ALU.add, op1=ALU.min),
                             reads=[("pGL", pu, 1), "buT"], writes=[("ltt", q)])
                        S.op("dve", lambda e, q=q: e.tensor_tensor(out=gtt[q][:], in0=gtt[q][:], in1=stt[q][:], op=ALU.mult), reads=[("gtt", q), ("stt", q)], writes=[("gtt", q)])
                        S.op("dve", lambda e, q=q, j=j, half=half: e.scalar_tensor_tensor(out=actT[:, j, half * 512:(half + 1) * 512], in0=ltt[q][:], scalar=-6.0, in1=gtt[q][:], op0=ALU.max, op1=ALU.mult),
                             reads=[("gtt", q), ("ltt", q)], writes=[("actT", j, half)])
                S.op("pool", lambda e, ex_=ex_: e.dma_start(out=bdr[:], in_=b_down[ex_:ex_ + 1, :]), writes=["bdr"], dma=True)
                for nt in range(8):
                    wb = wdring.next()
                    S.op("pool", lambda e, wb=wb, nt=nt, ex_=ex_: e.dma_start(out=wd[wb][:], in_=w_down[ex_, :, nt * 256:(nt + 1) * 256].rearrange("(c p) n -> p c n", p=P)),
                         writes=[("wd", wb)], dma=True)
                    for s in range(NS):
                        py = yring.next()
                        for fk in range(KC):
                            S.op("pe", lambda e, wb=wb, fk=fk, s=s, py=py: e.matmul(pY[py][:, 0:256], actT[:, fk, s * P:(s + 1) * P], wd[wb][:, fk, :], start=(fk == 0), stop=False),
                                 reads=[("wd", wb)] + [("actT", j, s // 4) for j in range(16)], writes=[("pY", py)])
                        S.op("pe", lambda e, nt=nt, py=py: e.matmul(pY[py][:, 0:256], onesb[:], bdr[:, nt * 256:(nt + 1) * 256], start=False, stop=True),
                             reads=["bdr", "onesb"], writes=[("pY", py)])
                        if ex_ == 0:
                            S.op("dve", lambda e, s=s, nt=nt, py=py, ex_=ex_: e.tensor_scalar(out=acc[:, s, nt * 256:(nt + 1) * 256], in0=pY[py][:, 0:256], scalar1=Wall[:, s, ex_:ex_ + 1], scalar2=None, op0=ALU.mult),
                                 reads=[("pY", py), "Wall"], writes=[("acc", s, nt // 2)])
                        else:
                            S.op("dve", lambda e, s=s, nt=nt, py=py, ex_=ex_: e.scalar_tensor_tensor(out=acc[:, s, nt * 256:(nt + 1) * 256], in0=pY[py][:, 0:256], scalar=Wall[:, s, ex_:ex_ + 1],
                                                                                                   in1=acc[:, s, nt * 256:(nt + 1) * 256], op0=ALU.mult, op1=ALU.add),
                                 reads=[("pY", py), "Wall", ("acc", s, nt // 2)], writes=[("acc", s, nt // 2)])
            S.emit()
        with ExitStack() as es:
            x2f = sb(es, "x2f", [P, KC, P])
            xf = sb(es, "xf", [P, D])
            of = [sb(es, f"of{i}", [P, D]) for i in range(2)]
            fss = sb(es, "fss", [P, 4])
            FGrow = sb(es, "FGrow", [P, D])
            pF = [ps(es, f"pF{i}", [P, 512]) for i in range(4)]
            S.op("sp", lambda e: e.dma_start(out=FGrow[:], in_=final_g.broadcast_to([P, D])), writes=["FGrow"], dma=True)
            for s in range(NS):
                b = s % 2
                S.op("sp", lambda e, s=s: e.dma_start(out=x2f[:], in_=X2T[:, :, s * P:(s + 1) * P].rearrange("c p t -> p c t")), writes=["x2f"], dma=True)
                for q in range(4):
                    for j in range(4):
                        kc = q * 4 + j
                        S.op("pe", lambda e, kc=kc, j=j, q=q: e.transpose(pF[q][:, j * P:(j + 1) * P], x2f[:, kc, :], ident), reads=["x2f", "cst"], writes=[("pF", q)])
                    S.op("dve", lambda e, s=s, q=q, b=b: e.tensor_tensor(out=of[b][:, q * 512:(q + 1) * 512], in0=acc[:, s, q * 512:(q + 1) * 512], in1=G2row[:, q * 512:(q + 1) * 512], op=ALU.mult),
                         reads=[("acc", s, q), "G2row"], writes=[("of", b)])
                    S.op("dve", lambda e, q=q, b=b: e.tensor_tensor(out=xf[:, q * 512:(q + 1) * 512], in0=pF[q][:], in1=of[b][:, q * 512:(q + 1) * 512], op=ALU.add),
                         reads=[("pF", q), ("of", b)], writes=["xf"])
                S.op("act", lambda e, b=b: e.activation(out=of[b][:], in_=xf[:], func=AF.Square, accum_out=fss[:, 0:1]), reads=["xf"], writes=[("of", b), "fss"])
                S.op("act", lambda e: e.activation(out=fss[:, 1:2], in_=fss[:, 0:1], func=AF.Sqrt, bias=epst[:], scale=1.0 / D), reads=["fss", "eps"], writes=["fss1"])
                S.op("dve", lambda e: e.reciprocal(out=fss[:, 2:3], in_=fss[:, 1:2]), reads=["fss1"], writes=["fss2"])
                S.op("dve", lambda e, b=b: e.scalar_tensor_tensor(out=of[b][:], in0=xf[:], scalar=fss[:, 2:3], in1=FGrow[:], op0=ALU.mult, op1=ALU.mult),
                     reads=["xf", "fss2", "FGrow"], writes=[("of", b)])
                S.op("sp", lambda e, s=s, b=b: e.dma_start(out=out[s * P:(s + 1) * P, :], in_=of[b][:]), reads=[("of", b)], writes=[("out", s)], dma=True)
            S.final_wait("sp", [("out", s) for s in range(NS)])
            S.emit()
    es_moe.close()
    es_glob.close()
    S.close()
    return nc


def _blk(j, i):
    return 8 * i + j if i % 2 == 0 else 8 * i + 7 - j


_CACHE = {}


def _prep(x, c, positions, w_ada, b_ada, norm1_g, w_in, conv_w, lambda_q1, lambda_k1,
          lambda_q2, lambda_k2, subln_g, w_attn_out, w_conv_out, w_o, norm2_g,
          w_router, b_router, w_up, b_up, w_down, b_down, final_g):
    f = lambda a: np.ascontiguousarray(np.asarray(a))
    x = f(x)[0]
    positions = f(positions)[0]
    n = 8
    tok = [np.concatenate([np.arange(_blk(j, i) * P, _blk(j, i) * P + P) for i in range(NS)]) for j in range(n)]
    tok_all = np.concatenate(tok)
    x_all = np.ascontiguousarray(x[tok_all])
    pos_all = np.ascontiguousarray(positions[tok_all].astype(np.int32))[None, :]
    cst = np.zeros((P, 1024), np.float32)
    cst[:, 0:128] = np.eye(P, dtype=np.float32)
    cst[:, 128:256] = 1.0
    cst[:, 256:384] = np.triu(np.ones((P, P), np.float32))
    cst[:, 384:640] = np.arange(256, dtype=np.float32)[None, :]
    inv_freq = (500000.0 ** (-np.arange(0, 32, 2, dtype=np.float32) / 32.0)).astype(np.float32)
    cst[0:32, 640] = np.tile(inv_freq, 2) / np.float32(2.0 * math.pi)
    rm = np.zeros((P, P), np.float32)
    for d in range(16):
        rm[d + 16, d] = -1.0
        rm[d, d + 16] = 1.0
    vecs = np.concatenate([f(norm1_g)[0].reshape(16, P), f(norm2_g)[0].reshape(16, P),
                           f(conv_w)[0].reshape(48, P), f(final_g).reshape(16, P)], 0).astype(np.float32)
    lamv = np.stack([np.concatenate([f(lambda_q1)[0], f(lambda_k1)[0]]), np.concatenate([f(lambda_q2)[0], f(lambda_k2)[0]])], 0).astype(np.float32)
    common = {
        "x_all": x_all, "pos_all": pos_all, "cst": cst, "rmat": rm,
        "c": f(c).reshape(16, P), "w_ada": f(w_ada)[0], "b_ada": f(b_ada), "vecs": vecs,
        "final_g": f(final_g)[None, :], "w_in": f(w_in)[0], "lamv": lamv, "subln_g": f(subln_g),
        "w_attn_out": f(w_attn_out)[0], "w_conv_out": f(w_conv_out)[0], "w_o": f(w_o)[0],
        "w_router": f(w_router)[0], "b_router": f(b_router), "w_up": f(w_up)[0],
        "b_up": f(b_up)[0].reshape(NE * 16, 256), "w_down": f(w_down)[0], "b_down": f(b_down)[0],
    }
    in_maps = []
    kk = np.arange(P)[:, None]
    qq = np.arange(P)[None, :]
    trimask = (kk <= qq).astype(np.float32)
    for j in range(n):
        m = np.zeros((P, 2, 8, P), np.float32)
        for r in range(8):
            m[:, 0, r, :] = 1.0 if r < j else (trimask if r == j else 0.0)
            m[:, 1, r, :] = 1.0 if r > j else (trimask if r == j else 0.0)
        xh = np.zeros((16, D), np.float32)
        hv = np.zeros((P, 16), np.float32)
        for i in range(NS):
            b = _blk(j, i)
            if b > 0:
                xh[2 * i:2 * i + 2] = x[b * P - 2:b * P]
                hv[:, 2 * i:2 * i + 2] = 1.0
        d = dict(common)
        d.update({"x_own": np.ascontiguousarray(x[tok[j]]), "x_halo": xh,
                  "pos_own": np.ascontiguousarray(positions[tok[j]].astype(np.int32))[None, :],
                  "masks": m, "halov": hv})
        in_maps.append(d)
    return in_maps, tok


def kernel(**inputs):
    in_maps, tok = _prep(**inputs)
    n = 8
    if "nc" not in _CACHE:
        _CACHE["nc"] = build_program()
    res = run_bass_kernel_spmd(_CACHE["nc"], in_maps, core_ids=list(range(n)))
    outp = np.zeros((8 * NT, D), np.float32)
    for j in range(n):
        outp[tok[j]] = np.asarray(res.results[j]["out"])
    return outp[None, :, :]
```
